# Optimizing a Trainium2 kernel written in Bass

```python
import math
import jax, jax.numpy as jnp
from jax import lax
import numpy as np

D_MODEL = 1024
BATCH = 2
SEQ = 8192
DEPTH = 1

CHUNK = 64
A_HEADS = 4
A_HEAD_DIM = 64
A_VDIM = 2 * A_HEAD_DIM
B_HEADS = 8
B_HEAD_DIM = 64
B_LEFT_CHUNKS = 8
B_MAX_REL = 128
T5_BUCKETS = 32
T5_MAX_DIST = 128
N_GROUPS = 4
EXPERTS_PER_GROUP = 8
N_EXPERTS = N_GROUPS * EXPERTS_PER_GROUP
TOP_K = 2
D_EXPERT = 512
MOE_BLOCK = 128
Q_BLOCK = 128
EPS = 1e-6

A_QK_W = A_HEADS * A_HEAD_DIM
A_V_W = A_HEADS * A_VDIM
B_W = B_HEADS * B_HEAD_DIM
PROJ_SIZES = [A_QK_W] * 4 + [A_V_W] + [B_W] * 3 + [D_MODEL] * 2
PROJ_W = sum(PROJ_SIZES)

kernel_name = "hybrid_diffattn_bandattn_hiermoe"


def rmsnorm(x, g):
    xf = x.astype(jnp.float32)
    y = xf * lax.rsqrt(jnp.mean(xf * xf, axis=-1, keepdims=True) + EPS)
    return (y * g.astype(jnp.float32)).astype(x.dtype)


def t5_bucket(rel):
    nb = T5_BUCKETS // 2
    max_exact = nb // 2
    side = jnp.where(rel > 0, nb, 0)
    n = jnp.abs(rel)
    nf = jnp.maximum(n, 1).astype(jnp.float32)
    large = max_exact + (jnp.log(nf / max_exact) / math.log(T5_MAX_DIST / max_exact)
                         * (nb - max_exact)).astype(jnp.int32)
    large = jnp.minimum(large, nb - 1)
    return side + jnp.where(n < max_exact, n, large)


def diff_attention(q1, q2, k1, k2, v, t5_table, lam):
    bsz, s_len, h, d = q1.shape
    nqb = s_len // Q_BLOCK
    scale = d ** -0.5
    k_pos = jnp.arange(s_len)

    def to_blocks(t):
        return t.reshape(bsz, nqb, Q_BLOCK, h, d).transpose(1, 0, 2, 3, 4)

    def block(args):
        qb1, qb2, bi = args
        q_pos = bi * Q_BLOCK + jnp.arange(Q_BLOCK)
        bias = t5_table[t5_bucket(k_pos[None, :] - q_pos[:, None])]
        bias = jnp.transpose(bias, (2, 0, 1)).astype(jnp.float32)
        allowed = (k_pos[None, :] // CHUNK) <= (q_pos[:, None] // CHUNK)

        def probs(qb, kk):
            s = jnp.einsum('bqhd,bkhd->bhqk', qb, kk).astype(jnp.float32) * scale + bias
            s = jnp.where(allowed, s, -1e30)
            return jax.nn.softmax(s, axis=-1)

        attn = probs(qb1, k1) - lam * probs(qb2, k2)
        return jnp.einsum('bhqk,bkhe->bqhe', attn.astype(v.dtype), v)

    out = lax.map(block, (to_blocks(q1), to_blocks(q2), jnp.arange(nqb)))
    return out.transpose(1, 0, 2, 3, 4).reshape(bsz, s_len, h, v.shape[-1])


def chunk_band_attention(q, k, v, rel_table):
    bsz, s_len, h, d = q.shape
    nc = s_len // CHUNK
    w = B_LEFT_CHUNKS
    band_len = (w + 1) * CHUNK
    qc = q.reshape(bsz, nc, CHUNK, h, d)

    def band(t):
        tp = jnp.pad(t, ((0, 0), (w * CHUNK, 0), (0, 0), (0, 0)))
        tp = tp.reshape(bsz, nc + w, CHUNK, h, t.shape[-1])
        return jnp.concatenate([tp[:, j:j + nc] for j in range(w + 1)], axis=2)

    kb, vb = band(k), band(v)
    qi = jnp.arange(CHUNK)
    kj = jnp.arange(band_len)
    rel = kj[None, :] - w * CHUNK - qi[:, None]
    rel_idx = jnp.clip(rel, -B_MAX_REL, B_MAX_REL) + B_MAX_REL
    bias = jnp.transpose(rel_table[rel_idx], (2, 0, 1)).astype(jnp.float32)
    valid = (jnp.arange(nc)[:, None] - w + kj[None, :] // CHUNK) >= 0
    s = jnp.einsum('bcqhd,bckhd->bchqk', qc, kb).astype(jnp.float32) * (d ** -0.5) + bias
    s = jnp.where(valid[None, :, None, None, :], s, -1e30)
    p = jax.nn.softmax(s, axis=-1)
    o = jnp.einsum('bchqk,bckhe->bcqhe', p.astype(v.dtype), vb)
    return o.reshape(bsz, s_len, h, d)


def hier_moe(x, w_rg, b_rg, w_re, b_re, w_gate, w_up, w_down):
    bsz, s_len, dm = x.shape
    n_tok = bsz * s_len
    xf = x.reshape(n_tok, dm)
    g_prob = jax.nn.softmax((xf @ w_rg).astype(jnp.float32) + b_rg.astype(jnp.float32), axis=-1)
    p_g, g_idx = lax.top_k(g_prob, 1)
    e_logits = ((xf @ w_re).astype(jnp.float32) + b_re.astype(jnp.float32)).reshape(n_tok, N_GROUPS, EXPERTS_PER_GROUP)
    e_in_group = jnp.take_along_axis(e_logits, g_idx[:, :, None], axis=1)[:, 0]
    top_l, top_j = lax.top_k(e_in_group, TOP_K)
    comb = p_g * jax.nn.softmax(top_l, axis=-1)
    expert_id = g_idx * EXPERTS_PER_GROUP + top_j

    n_assign = n_tok * TOP_K
    e_flat = expert_id.reshape(n_assign)
    tok_flat = jnp.repeat(jnp.arange(n_tok, dtype=jnp.int32), TOP_K)
    w_flat = comb.reshape(n_assign)
    order = jnp.argsort(e_flat)
    se, stok, sw = e_flat[order], tok_flat[order], w_flat[order]
    counts = jax.ops.segment_sum(jnp.ones((n_assign,), jnp.int32), e_flat, num_segments=N_EXPERTS)
    padded = ((counts + MOE_BLOCK - 1) // MOE_BLOCK) * MOE_BLOCK
    start = jnp.cumsum(counts) - counts
    pend = jnp.cumsum(padded)
    pstart = pend - padded
    dest = pstart[se] + (jnp.arange(n_assign) - start[se])
    n_blocks = (n_assign + MOE_BLOCK - 1) // MOE_BLOCK + N_EXPERTS
    n_rows = n_blocks * MOE_BLOCK
    buf_tok = jnp.zeros((n_rows,), jnp.int32).at[dest].set(stok)
    buf_w = jnp.zeros((n_rows,), jnp.float32).at[dest].set(sw)
    block_expert = jnp.minimum(jnp.searchsorted(pend, jnp.arange(n_blocks) * MOE_BLOCK, side='right'),
                               N_EXPERTS - 1)

    def run_block(args):
        tok, e = args
        xb = xf[tok]
        hb = jax.nn.silu(xb @ w_gate[e]) * (xb @ w_up[e])
        return hb @ w_down[e]

    yb = lax.map(run_block, (buf_tok.reshape(n_blocks, MOE_BLOCK), block_expert))
    yb = yb.reshape(n_rows, dm) * buf_w[:, None].astype(yb.dtype)
    y = jnp.zeros((n_tok, dm), x.dtype).at[buf_tok].add(yb.astype(x.dtype))
    return y.reshape(bsz, s_len, dm)


def setup_inputs(seed: int = 0) -> dict:
    key = jax.random.key(seed)
    ks = jax.random.split(key, 24)
    f = jnp.float32

    def nrm(k, shape, scale):
        return jax.random.normal(k, shape, f) * scale

    def gain(k, shape):
        return 1.0 + 0.05 * jax.random.normal(k, shape, f)

    L = DEPTH
    return {
        "x": nrm(ks[0], (BATCH, SEQ, D_MODEL), 1.0),
        "norm1_g": gain(ks[1], (L, D_MODEL)),
        "w_in": nrm(ks[2], (L, D_MODEL, PROJ_W), D_MODEL ** -0.5),
        "a_qnorm_g": gain(ks[3], (L, A_HEAD_DIM)),
        "a_knorm_g": gain(ks[4], (L, A_HEAD_DIM)),
        "a_lambda": nrm(ks[5], (L, 4, A_HEAD_DIM), 0.1),
        "a_subln_g": gain(ks[6], (L, A_VDIM)),
        "t5_table": nrm(ks[7], (T5_BUCKETS, A_HEADS), 0.5),
        "b_qnorm_g": gain(ks[8], (L, B_HEAD_DIM)),
        "b_knorm_g": gain(ks[9], (L, B_HEAD_DIM)),
        "b_rel_table": nrm(ks[10], (L, 2 * B_MAX_REL + 1, B_HEADS), 0.5),
        "w_branch_a": nrm(ks[11], (L, A_V_W, D_MODEL), A_V_W ** -0.5),
        "w_branch_b": nrm(ks[12], (L, B_W, D_MODEL), B_W ** -0.5),
        "w_out": nrm(ks[13], (L, D_MODEL, D_MODEL), D_MODEL ** -0.5),
        "norm2_g": gain(ks[14], (L, D_MODEL)),
        "w_router_group": nrm(ks[15], (L, D_MODEL, N_GROUPS), D_MODEL ** -0.5),
        "b_router_group": nrm(ks[16], (L, N_GROUPS), 0.01),
        "w_router_expert": nrm(ks[17], (L, D_MODEL, N_EXPERTS), D_MODEL ** -0.5),
        "b_router_expert": nrm(ks[18], (L, N_EXPERTS), 0.01),
        "w_gate": nrm(ks[19], (L, N_EXPERTS, D_MODEL, D_EXPERT), D_MODEL ** -0.5),
        "w_up": nrm(ks[20], (L, N_EXPERTS, D_MODEL, D_EXPERT), D_MODEL ** -0.5),
        "w_down": nrm(ks[21], (L, N_EXPERTS, D_EXPERT, D_MODEL), D_EXPERT ** -0.5),
    }


def reference(x, norm1_g, w_in, a_qnorm_g, a_knorm_g, a_lambda, a_subln_g, t5_table,
              b_qnorm_g, b_knorm_g, b_rel_table, w_branch_a, w_branch_b, w_out, norm2_g,
              w_router_group, b_router_group, w_router_expert, b_router_expert,
              w_gate, w_up, w_down):
    bsz, s_len, _ = x.shape
    split_idx = np.cumsum(PROJ_SIZES)[:-1].tolist()

    def heads(t, h):
        return t.reshape(bsz, s_len, h, -1)

    for l in range(DEPTH):
        xn = rmsnorm(x, norm1_g[l])
        proj = xn @ w_in[l]
        q1, q2, k1, k2, va, qb, kb, vb, gate_a, gate_b = jnp.split(proj, split_idx, axis=-1)

        q1 = rmsnorm(heads(q1, A_HEADS), a_qnorm_g[l])
        q2 = rmsnorm(heads(q2, A_HEADS), a_qnorm_g[l])
        k1 = rmsnorm(heads(k1, A_HEADS), a_knorm_g[l])
        k2 = rmsnorm(heads(k2, A_HEADS), a_knorm_g[l])
        va = heads(va, A_HEADS)
        lam_init = 0.8 - 0.6 * math.exp(-0.3 * l)
        lp = a_lambda[l].astype(jnp.float32)
        lam = jnp.exp(jnp.sum(lp[0] * lp[1])) - jnp.exp(jnp.sum(lp[2] * lp[3])) + lam_init
        oa = diff_attention(q1, q2, k1, k2, va, t5_table, lam)
        oa = rmsnorm(oa, a_subln_g[l]) * (1.0 - lam_init)
        ya = oa.reshape(bsz, s_len, A_V_W) @ w_branch_a[l]

        qb = rmsnorm(heads(qb, B_HEADS), b_qnorm_g[l])
        kb = rmsnorm(heads(kb, B_HEADS), b_knorm_g[l])
        vb = heads(vb, B_HEADS)
        ob = chunk_band_attention(qb, kb, vb, b_rel_table[l])
        yb = ob.reshape(bsz, s_len, B_W) @ w_branch_b[l]

        mixed = jax.nn.sigmoid(gate_a) * ya + jax.nn.sigmoid(gate_b) * yb
        x = x + mixed @ w_out[l]

        hn = rmsnorm(x, norm2_g[l])
        x = x + hier_moe(hn, w_router_group[l], b_router_group[l], w_router_expert[l],
                         b_router_expert[l], w_gate[l], w_up[l], w_down[l])
    return x
```

```python
import math
from contextlib import ExitStack

import numpy as np
import concourse.bass as bass
import concourse.mybir as mybir
from concourse.bass_utils import run_bass_kernel_spmd

F32 = mybir.dt.float32
BF16 = mybir.dt.bfloat16
U32 = mybir.dt.uint32
AF = mybir.ActivationFunctionType
ALU = mybir.AluOpType
AX = mybir.AxisListType

D = 1024
SEQ = 8192
NCORE = 8
OWN = 2048
CAP = 256
NEXP = 32
EPS = 1e-6
NEG = -30000.0
NSLOT_A = 5
NSLOT_B = 8
DEBUG = None


class Sched:
    NDS = 48

    def __init__(self, nc, es):
        self.nc = nc
        self.eng = {"pe": nc.tensor, "act": nc.scalar, "dve": nc.vector, "pool": nc.gpsimd, "sp": nc.sync}
        self.sem = {k: es.enter_context(nc.semaphore("sem_" + k)) for k in ("pe", "act", "dve", "pool")}
        self.cnt = {k: 0 for k in self.sem}
        self.dsem = [es.enter_context(nc.semaphore("dsem%d" % i)) for i in range(self.NDS)]
        self.dcnt = [0] * self.NDS
        self.drr = 0
        self.waited = {}
        self.last_w = {}
        self.readers = {}
        self.ninstr = 0
        self.pend = {k: False for k in self.sem}

    def _semobj(self, key):
        return self.dsem[key[1]] if isinstance(key, tuple) else self.sem[key]

    def _wait(self, e, ev):
        key, val = ev
        if self.waited.get((e, key), 0) >= val:
            return
        self.eng[e].wait_ge(self._semobj(key), val)
        self.waited[(e, key)] = val

    def op(self, e, fn, reads=(), writes=(), dma=False, sig=True):
        need = {}

        def add(ev):
            if ev is not None and need.get(ev[0], 0) < ev[1]:
                need[ev[0]] = ev[1]

        for b in reads:
            add(self.last_w.get(b))
        for b in writes:
            add(self.last_w.get(b))
            for k, v in self.readers.get(b, {}).items():
                add((k, v))
        if dma:
            k = self.drr
            self.drr = (self.drr + 1) % self.NDS
            if self.dcnt[k]:
                add((("d", k), self.dcnt[k]))
        for key, val in need.items():
            if key == "pe" and e == "pe":
                continue
            self._wait(e, (key, val))
        ins = fn()
        self.ninstr += 1
        if dma:
            self.dcnt[k] += 16
            ins.then_inc(self.dsem[k], 16)
            ev = (("d", k), self.dcnt[k])
        elif sig:
            self.cnt[e] += 1
            ins.then_inc(self.sem[e], 1)
            ev = (e, self.cnt[e])
            self.pend[e] = False
        else:
            ev = (e, self.cnt[e] + 1)
            self.pend[e] = True
        for b in reads:
            r = self.readers.setdefault(b, {})
            if r.get(ev[0], 0) < ev[1]:
                r[ev[0]] = ev[1]
        for b in writes:
            self.last_w[b] = ev
            self.readers[b] = {}
        return ev

    def barrier(self):
        assert not any(self.pend.values()), self.pend
        for e in self.eng:
            for k in self.sem:
                if self.cnt[k]:
                    self._wait(e, (k, self.cnt[k]))
            for i in range(self.NDS):
                if self.dcnt[i]:
                    self._wait(e, (("d", i), self.dcnt[i]))

    def wait_evs(self, e, evs):
        for ev in evs:
            self._wait(e, ev)

    def wait_all(self, e, bufs):
        for b in bufs:
            ev = self.last_w.get(b)
            if ev is not None:
                self._wait(e, ev)


def _t5_bucket(rel):
    nb = 16
    max_exact = 8
    side = np.where(rel > 0, nb, 0)
    n = np.abs(rel)
    nf = np.maximum(n, 1).astype(np.float32)
    large = max_exact + (np.log(nf / np.float32(max_exact)) / np.float32(math.log(128 / max_exact))
                         * np.float32(nb - max_exact)).astype(np.int32)
    large = np.minimum(large, nb - 1)
    return side + np.where(n < max_exact, n, large)


def _structs(j):
    u = np.arange(256)
    kk = np.arange(128)[:, None]
    qq = np.arange(128)[None, :]
    chunk_ok = (kk // 64) <= (qq // 64)
    ohA = np.zeros((32, NSLOT_A, 256), np.float32)
    mA = np.zeros((128, NSLOT_A, 128), np.float32)
    for sl in range(NSLOT_A):
        dl = j - (sl - 1)
        if dl < 0:
            mA[:, sl, :] = NEG
            continue
        rel = 127 - u - 128 * dl
        bk = _t5_bucket(rel)
        ohA[bk, sl, u] += 1.0
        ohA[15, sl, u] -= 1.0
        ohA[:, sl, 255] = 0.0
        if dl == 0:
            mA[:, sl, :] = np.where(chunk_ok, 0.0, NEG)
    ohB = np.zeros((256, NSLOT_B, 256), np.float32)
    mB = np.zeros((128, NSLOT_B, 128), np.float32)
    for sl in range(NSLOT_B):
        dl = j + 4 - sl
        if dl < 0 or dl > 4:
            mB[:, sl, :] = NEG
            continue
        rel = 127 - u - 128 * dl
        idx = np.clip(rel, -128, 128) + 128
        ohB[idx, sl, u] += 1.0
        ohB[0, sl, u] -= 1.0
        ohB[:, sl, 255] = 0.0
        if dl == 0:
            mB[:, sl, :] = np.where(chunk_ok, 0.0, NEG)
        if dl == 4:
            mB[:, sl, :] = np.where((qq >= 64) & (kk < 64), NEG, 0.0)
    return (ohA.reshape(32, NSLOT_A * 256), mA,
            ohB.reshape(2, 128, NSLOT_B * 256).transpose(1, 0, 2).copy(), mB)


def _consts():
    ident = np.eye(128, dtype=np.float32)
    J = ident[::-1].copy()
    bd = np.zeros((128, 128), np.float32)
    bd[:64, :64] = 1.0 / 64
    bd[64:, 64:] = 1.0 / 64
    U = np.triu(np.ones((128, 128), np.float32), 1)
    return ident, J, bd, U


def build_program():
    nc = bass.Bass("TRN2", target_bir_lowering=False)

    def din(name, shape, dt=F32):
        return nc.dram_tensor(name, list(shape), dt, kind="ExternalInput").ap()

    xT_all = din("xT_all", [D, SEQ])
    xT_own = din("xT_own", [D, OWN])
    x_own = din("x_own", [OWN, D])
    w_in = din("w_in", [D, 5120])
    norm1_g = din("norm1_g", [1, D])
    a_qg = din("a_qnorm_g", [1, 64])
    a_kg = din("a_knorm_g", [1, 64])
    a_lambda = din("a_lambda", [1, 256])
    a_subg = din("a_subln_g", [1, 128])
    t5_table = din("t5_table", [32, 4])
    b_qg = din("b_qnorm_g", [1, 64])
    b_kg = din("b_knorm_g", [1, 64])
    b_rel = din("b_rel_table", [257, 8])
    w_ba = din("w_branch_a", [512, D])
    w_bb = din("w_branch_b", [512, D])
    w_out = din("w_out", [D, D])
    norm2_g = din("norm2_g", [1, D])
    w_rg = din("w_router_group", [D, 4])
    b_rg = din("b_router_group", [1, 4])
    w_re = din("w_router_expert", [D, 32])
    b_re = din("b_router_expert", [1, 32])
    w_gate = din("w_gate", [NEXP, D, 512])
    w_up = din("w_up", [NEXP, D, 512])
    w_down = din("w_down", [NEXP, 512, D])
    c_ident = din("c_ident", [128, 128])
    c_J = din("c_J", [128, 128])
    c_bd = din("c_bd", [128, 128])
    c_U = din("c_U", [128, 128])
    c_ohA = din("c_ohA", [32, NSLOT_A * 256])
    c_mA = din("c_mA", [128, NSLOT_A, 128])
    c_ohB = din("c_ohB", [128, 2, NSLOT_B * 256])
    c_mB = din("c_mB", [128, NSLOT_B, 128])
    out_own = nc.dram_tensor("out_own", [OWN, D], F32, kind="ExternalOutput").ap()

    def dscr(name, shape, dt):
        return nc.dram_tensor(name, list(shape), dt, kind="Internal")

    KT_t = dscr("KT", [8, 128, SEQ], BF16)
    VA_t = dscr("VA", [4, 128, 64, 130], BF16)
    VB_t = dscr("VB", [4, 128, 64, 132], BF16)
    FA_t = dscr("FA", [4, NSLOT_A * 256], F32)
    FB_t = dscr("FB", [8, NSLOT_B * 256], F32)
    XS_t = dscr("XS", [NEXP * CAP, D], BF16)
    YS_t = dscr("YS", [NEXP * CAP, D], BF16)
    XM_t = dscr("XM", [OWN, D], F32)
    WG16 = dscr("WG16", [NEXP, 128, 8 * 512], BF16).ap()
    WU16 = dscr("WU16", [NEXP, 128, 8 * 512], BF16).ap()
    WD16 = dscr("WD16", [NEXP, 128, 4 * D], BF16).ap()
    KT, VA, VB, FA, FB, XS, YS, XM = (t.ap() for t in (KT_t, VA_t, VB_t, FA_t, FB_t, XS_t, YS_t, XM_t))

    with ExitStack() as top:
        top.enter_context(nc.allow_non_contiguous_dma(reason="tiny parameter vectors"))
        S = Sched(nc, top)
        PE, ACT, DVE, POOL, SP = nc.tensor, nc.scalar, nc.vector, nc.gpsimd, nc.sync

        def sb(es, name, shape, dt):
            return es.enter_context(nc.sbuf_tensor(name, list(shape), dt))

        def ps(es, name, shape, dt=F32):
            return es.enter_context(nc.psum_tensor(name, list(shape), dt))

        def ld(out_ap, in_ap, reads, writes, cast=False):
            if cast:
                return S.op("pool", lambda: POOL.dma_start(out=out_ap, in_=in_ap), reads, writes, dma=True)
            return S.op("sp", lambda: SP.dma_start(out=out_ap, in_=in_ap), reads, writes, dma=True)

        ident_f = sb(top, "ident_f", [128, 128], F32)
        ident_b = sb(top, "ident_b", [128, 128], BF16)
        ones_b = sb(top, "ones_b", [128, 128], BF16)
        bd_b = sb(top, "bd_b", [128, 128], BF16)
        g1 = sb(top, "g1", [128, 8], F32)
        gq = sb(top, "gq", [128, 4], F32)
        gsub = sb(top, "gsub", [128, 128], F32)
        neglam = sb(top, "neglam", [128, 1], F32)
        mhalf = sb(top, "mhalf", [128, 1], F32)
        EPS_T = sb(top, "eps_t", [128, 1], F32)
        mid = top.enter_context(ExitStack())
        OAT = sb(mid, "OAT", [128, 4, OWN], BF16)
        OBT = sb(mid, "OBT", [128, 4, OWN], BF16)
        qs = mid.enter_context(ExitStack())
        QT = sb(qs, "QT", [128, 8, OWN], BF16)

        ld(ident_f[:], c_ident, [], ["ident_f"])
        S.op("dve", lambda: DVE.tensor_copy(out=ident_b[:], in_=ident_f[:]), ["ident_f"], ["ident_b"])
        S.op("dve", lambda: DVE.memset(ones_b[:], 1.0), [], ["ones_b"])
        S.op("dve", lambda: DVE.memset(mhalf[:], -0.5), [], ["mhalf"])
        ld(g1[:], norm1_g.rearrange("o (c p) -> p (o c)", p=128), [], ["g1"])
        for col, src in enumerate((a_qg, a_kg, b_qg, b_kg)):
            for hh in range(2):
                ld(gq[hh * 64:(hh + 1) * 64, col:col + 1], src.rearrange("o d -> d o"), [], ["gq"])
        for col in (0, 2):
            S.op("dve", lambda col=col: DVE.tensor_scalar(out=gq[:, col:col + 1], in0=gq[:, col:col + 1],
                                                          scalar1=0.125, scalar2=None, op0=ALU.mult),
                 ["gq"], ["gq"])
        ld(gsub[:], a_subg.partition_broadcast(128).rearrange("p o d -> p (o d)"), [], ["gsub"])
        S.op("dve", lambda: DVE.tensor_scalar(out=gsub[:], in0=gsub[:], scalar1=0.8, scalar2=None, op0=ALU.mult),
             ["gsub"], ["gsub"])

        def norm_pieces(es_bufs, src_ap, tag):
            xT, xsq, xn, rtmp, rstd, pss = es_bufs

            def p_load():
                ld(xT[:], src_ap.rearrange("(c p) t -> p c t", p=128), [], [("xT", tag)], cast=True)
                S.op("act", lambda: ACT.activation(out=xsq[:], in_=xT[:], func=AF.Square), [("xT", tag)], [("xsq", tag)])

            def p_stat():
                for c in range(8):
                    S.op("pe", lambda c=c: PE.matmul(pss[:], lhsT=ones_b[:], rhs=xsq[:, c, :], start=(c == 0), stop=(c == 7)),
                         [("xsq", tag), "ones_b"], ["pss"], sig=(c == 7))
                S.op("act", lambda: ACT.activation(out=rtmp[:], in_=pss[:], func=AF.Ln, bias=EPS_T[:], scale=1.0 / D),
                     ["pss"], [("rtmp", tag)])
                S.op("act", lambda: ACT.activation(out=rstd[:], in_=rtmp[:], func=AF.Exp, scale=-0.5), [("rtmp", tag)], [("rstd", tag)])

            def p_xn(c):
                def f():
                    S.op("dve", lambda: DVE.scalar_tensor_tensor(out=xn[:, c, :], in0=xT[:, c, :], scalar=g1[:, c:c + 1],
                                                                 in1=rstd[:], op0=ALU.mult, op1=ALU.mult),
                         [("xT", tag), ("rstd", tag), "g1"], [("xn", tag, c)])
                return f

            return [p_load, p_stat] + [p_xn(c) for c in range(8)], xn

        def norm_group(es_bufs, src_ap, tag):
            pieces, xn = norm_pieces(es_bufs, src_ap, tag)
            for p in pieces:
                p()
            return xn

        S.op("dve", lambda: DVE.memset(EPS_T[:], EPS), [], ["eps_t"])

        def precast(e, pace):
            ld(WG16[e].rearrange("p (c n) -> p c n", n=512), w_gate[e].rearrange("(c p) n -> p c n", p=128),
               pace, [("W16", e, 0)], cast=True)
            ld(WU16[e].rearrange("p (c n) -> p c n", n=512), w_up[e].rearrange("(c p) n -> p c n", p=128),
               pace, [("W16", e, 1)], cast=True)
            for hf in range(2):
                ld(WD16[e].rearrange("p (c n) -> p c n", n=1024)[:, :, hf * 512:(hf + 1) * 512],
                   w_down[e][:, hf * 512:(hf + 1) * 512].rearrange("(c p) n -> p c n", p=128),
                   pace, [("W16", e, 2, hf)], cast=True)

        with ExitStack() as es:
            wk = sb(es, "wk", [128, 8, 1024], BF16)
            wv = sb(es, "wv", [128, 8, 1024], BF16)
            wq = sb(es, "wq", [128, 8, 1024], BF16)
            xTb = [sb(es, "xT%d" % i, [128, 8, 512], BF16) for i in range(2)]
            xsqb = [sb(es, "xsq%d" % i, [128, 8, 512], BF16) for i in range(2)]
            xnb = [sb(es, "xn%d" % i, [128, 8, 512], BF16) for i in range(2)]
            rtmpb = [sb(es, "rtmp%d" % i, [128, 512], F32) for i in range(2)]
            rstdb = [sb(es, "rstd%d" % i, [128, 512], F32) for i in range(2)]
            sqb = [sb(es, "sqb%d" % i, [128, 512], BF16) for i in range(3)]
            rt = [sb(es, "rt%d" % i, [128, 512], F32) for i in range(2)]
            rk = [sb(es, "rk%d" % i, [128, 512], F32) for i in range(2)]
            kto = [sb(es, "kto%d" % i, [128, 512], BF16) for i in range(4)]
            vstA = [sb(es, "vstA%d" % i, [128, 4, 4, 130], BF16) for i in range(2)]
            vstB = [sb(es, "vstB%d" % i, [128, 4, 4, 132], BF16) for i in range(2)]
            pss = ps(es, "pss", [128, 512])
            psr = [ps(es, "psr%d" % i, [128, 512]) for i in range(3)]
            psm = [ps(es, "psm%d" % i, [128, 512]) for i in range(2)]
            psv = [ps(es, "psv%d" % i, [128, 512]) for i in range(2)]

            wevs = []

            def wload(dst, col0, src_col0, ncols):
                wevs.append(ld(dst[:, :, col0:col0 + ncols],
                               w_in[:, src_col0:src_col0 + ncols].rearrange("(c p) n -> p c n", p=128), [], [], cast=True))

            pcs_first, xn_first = norm_pieces((xTb[0], xsqb[0], xnb[0], rtmpb[0], rstdb[0], pss), xT_all[:, 0:512], 0)
            pcs_first[0]()
            wload(wk, 0, 512, 512)
            wload(wk, 512, 2048, 512)
            wload(wv, 0, 1024, 512)
            wload(wv, 512, 2560, 512)
            wload(wq, 0, 0, 512)
            wload(wq, 512, 1536, 512)
            for i in range(2):
                S.op("dve", lambda i=i: DVE.memset(vstA[i][:], 1.0), [], [("vstA", i)])
                S.op("dve", lambda i=i: DVE.memset(vstB[i][:], 1.0), [], [("vstB", i)])
            groups = [("k", g) for g in range(16)] + [("q", g) for g in range(4)]
            with ExitStack() as es:
                bd_f = sb(es, "bd_f", [128, 128], F32)
                lamt = sb(es, "lamt", [128, 256], F32)
                lprod = sb(es, "lprod", [128, 2, 64], F32)
                lsum = sb(es, "lsum", [128, 2], F32)
                lexp = sb(es, "lexp", [128, 2], F32)
                ld(bd_f[:], c_bd, [], ["bd_f"])
                S.op("dve", lambda: DVE.tensor_copy(out=bd_b[:], in_=bd_f[:]), ["bd_f"], ["bd_b"])
                ld(lamt[:], a_lambda.partition_broadcast(128).rearrange("p o d -> p (o d)"), [], ["lamt"])
                S.op("dve", lambda: DVE.tensor_tensor(out=lprod[:, 0, :], in0=lamt[:, 0:64], in1=lamt[:, 64:128], op=ALU.mult),
                     ["lamt"], ["lprod0"])
                S.op("dve", lambda: DVE.tensor_tensor(out=lprod[:, 1, :], in0=lamt[:, 128:192], in1=lamt[:, 192:256], op=ALU.mult),
                     ["lamt"], ["lprod1"])
                S.op("dve", lambda: DVE.tensor_reduce(out=lsum[:], in_=lprod[:], axis=AX.X, op=ALU.add),
                     ["lprod0", "lprod1"], ["lsum"])
                S.op("act", lambda: ACT.activation(out=lexp[:], in_=lsum[:], func=AF.Exp), ["lsum"], ["lexp"])
                S.op("dve", lambda: DVE.tensor_tensor(out=neglam[:], in0=lexp[:, 1:2], in1=lexp[:, 0:1], op=ALU.subtract),
                     ["lexp"], ["neglam"])
                S.op("dve", lambda: DVE.tensor_scalar(out=neglam[:], in0=neglam[:], scalar1=-0.2, scalar2=None, op0=ALU.add),
                     ["neglam"], ["neglam"])
                S.barrier()
            S.wait_evs("pe", wevs)

            def norm_of(gi):
                kind, g = groups[gi]
                src = xT_all if kind == "k" else xT_own
                p = gi % 2
                return norm_pieces((xTb[p], xsqb[p], xnb[p], rtmpb[p], rstdb[p], pss), src[:, g * 512:(g + 1) * 512], p)

            nxt_pieces = [[]]
            slot_of_piece = [0, 3, 4, 5, 6, 7, 8, 9, 10, 11]
            tcount = [0]

            def pump():
                k = tcount[0]
                tcount[0] += 1
                while nxt_pieces[0] and nxt_pieces[0][0][0] <= k:
                    nxt_pieces[0].pop(0)[1]()

            pend = [None]

            def task(p1, p2):
                pump()
                p1()
                if pend[0] is not None:
                    pend[0]()
                pend[0] = p2

            cnt = [0]

            def qk_task(wt, wcol, xn, tag, gcol, out_ap, out_keys, after=None):
                i3 = cnt[0] % 3
                i2 = cnt[0] % 2
                cnt[0] += 1
                pr, pm = psr[i3], psm[i2]

                def p1():
                    for c in range(8):
                        S.op("pe", lambda c=c: PE.matmul(pr[:], lhsT=wt[:, c, wcol:wcol + 128], rhs=xn[:, c, :],
                                                         start=(c == 0), stop=(c == 7)),
                             [("xn", tag, c)], [("psr", i3)], sig=(c == 7))
                    S.op("act", lambda: ACT.activation(out=sqb[i3][:], in_=pr[:], func=AF.Square),
                         [("psr", i3)], [("sqb", i3)])

                def p2():
                    S.op("pe", lambda: PE.matmul(pm[:], lhsT=bd_b[:], rhs=sqb[i3][:], start=True, stop=True),
                         [("sqb", i3)], [("psm", i2)])
                    S.op("act", lambda: ACT.activation(out=rt[i2][:], in_=pm[:], func=AF.Ln, bias=EPS_T[:], scale=1.0),
                         [("psm", i2)], [("rt", i2)])
                    S.op("act", lambda: ACT.activation(out=rk[i2][:], in_=rt[i2][:], func=AF.Exp, scale=-0.5),
                         [("rt", i2)], [("rk", i2)])
                    S.op("dve", lambda: DVE.scalar_tensor_tensor(out=out_ap, in0=pr[:], scalar=gq[:, gcol:gcol + 1],
                                                                 in1=rk[i2][:], op0=ALU.mult, op1=ALU.mult),
                         [("psr", i3), ("rk", i2)], out_keys)
                    if after is not None:
                        after()

                task(p1, p2)

            vcnt = [0]

            def v_task(xn, tag, tt, half, vsA, vsB, vkeyA, vkeyB, after=None):
                iv = vcnt[0] % 2
                vcnt[0] += 1
                pv = psv[iv]
                key = ("psv", iv)

                def p1():
                    for c in range(8):
                        S.op("pe", lambda c=c: PE.matmul(
                            pv[:], lhsT=xn[:, c, tt * 128:(tt + 1) * 128], rhs=wv[:, c, half * 512:(half + 1) * 512],
                            start=(c == 0), stop=(c == 7)), [("xn", tag, c)], [key], sig=(c == 7))

                def p2():
                    if half == 0:
                        S.op("act", lambda: ACT.activation(
                            out=vsA[:, :, tt, 0:128], in_=pv[:].rearrange("p (h d) -> p h d", d=128), func=AF.Copy),
                            [key], [vkeyA])
                    else:
                        S.op("dve", lambda: DVE.tensor_copy(
                            out=vsB[:, :, tt, :].rearrange("p a (h d) -> p a h d", d=66)[:, :, :, 0:64],
                            in_=pv[:].rearrange("p (a h d) -> p a h d", h=2, d=64)), [key], [vkeyB])
                    if after is not None:
                        after()

                task(p1, p2)

            xn_cur = xn_first
            for p_ in pcs_first[1:]:
                p_()
            kcount = 0
            for gi, (kind, g) in enumerate(groups):
                xn_next = None
                tcount[0] = 0
                if gi + 1 < len(groups):
                    pcs, xn_next = norm_of(gi + 1)
                    nxt_pieces[0] = list(zip(slot_of_piece, pcs))
                tag = gi % 2
                if gi < 16 and gi % 2 == 0:
                    precast(gi // 2, [("xT", gi % 2)])
                if kind == "k":
                    for kc in range(8):
                        ko = kto[kcount % 4]
                        kkey = ("kto", kcount % 4)
                        kcount += 1

                        def st(ko=ko, kkey=kkey, kc=kc, g=g):
                            ld(KT[kc, :, g * 512:(g + 1) * 512], ko[:], [kkey], [("KT", kc, g)])

                        qk_task(wk, kc * 128, xn_cur, tag, 1 if kc < 4 else 3, ko[:], [kkey], after=st)
                    vsA, vsB = vstA[g % 2], vstB[g % 2]
                    for tt in range(4):
                        for half in range(2):
                            aft = None
                            if tt == 3 and half == 0:
                                def aft(vsA=vsA, g=g):
                                    ld(VA[:, :, g * 4:(g + 1) * 4, :].rearrange("h p t d -> p h (t d)"),
                                       vsA[:].rearrange("p h t d -> p h (t d)"), [("vstA", g % 2)], [("VA", g)])
                            if tt == 3 and half == 1:
                                def aft(vsB=vsB, g=g):
                                    ld(VB[:, :, g * 4:(g + 1) * 4, :].rearrange("h p t d -> p h (t d)"),
                                       vsB[:].rearrange("p h t d -> p h (t d)"), [("vstB", g % 2)], [("VB", g)])
                            v_task(xn_cur, tag, tt, half, vsA, vsB, ("vstA", g % 2), ("vstB", g % 2), after=aft)
                else:
                    for qc in range(8):
                        qk_task(wq, qc * 128, xn_cur, tag, 0 if qc < 4 else 2, QT[:, qc, g * 512:(g + 1) * 512], ["QT"])
                while nxt_pieces[0]:
                    nxt_pieces[0].pop(0)[1]()
                xn_cur = xn_next
            if pend[0] is not None:
                pend[0]()
                pend[0] = None
            S.barrier()

        if DEBUG == "K":
            dbg = nc.dram_tensor("dbg", [128, 8, OWN], BF16, kind="ExternalOutput").ap()
            ev = ld(dbg, QT[:], ["QT"], ["dbg"])
            S.wait_all("sp", ["dbg"])
            return nc

        slotA = sb(qs, "slotA", [128, 4, NSLOT_A, 128], BF16)
        slotB = sb(qs, "slotB", [128, 8, NSLOT_B, 128], BF16)
        with ExitStack() as es:
            J_f = sb(es, "J_f", [128, 128], F32)
            tabA = sb(es, "tabA", [32, 4], F32)
            ohA = sb(es, "ohA", [32, NSLOT_A * 256], F32)
            mA = sb(es, "mA", [128, NSLOT_A, 128], F32)
            tabB = sb(es, "tabB", [128, 2, 8], F32)
            ohB = sb(es, "ohB", [128, 2, NSLOT_B * 256], F32)
            mB = sb(es, "mB", [128, NSLOT_B, 128], F32)
            Fsb = sb(es, "Fsb", [8, NSLOT_B * 256], F32)
            stage = sb(es, "stage", [128, NSLOT_B, 128], F32)
            psF = ps(es, "psF", [8, 512])
            psT = [ps(es, "psT%d" % i, [128, 512]) for i in range(2)]

            ld(J_f[:], c_J, [], ["J_f"])

            def build_slots(tab_parts, oh_parts, H, nsl, Fdram, Ft, mask, slot):
                ncol = nsl * 256
                for c0 in range(0, ncol, 512):
                    w = min(512, ncol - c0)
                    for i, (tp, op_) in enumerate(zip(tab_parts, oh_parts)):
                        S.op("pe", lambda tp=tp, op_=op_, c0=c0, w=w, i=i: PE.matmul(
                            psF[0:H, 0:w], lhsT=tp, rhs=op_[:, c0:c0 + w], start=(i == 0), stop=(i == len(tab_parts) - 1)),
                            ["tab", "oh"], ["psF"])
                    S.op("dve", lambda c0=c0, w=w: DVE.tensor_copy(out=Fsb[0:H, c0:c0 + w], in_=psF[0:H, 0:w]),
                         ["psF"], ["Fsb"])
                ld(Fdram, Fsb[0:H, 0:ncol], ["Fsb"], ["Fdram"])
                for h in range(H):
                    hank = bass.AP(tensor=Ft, offset=h * ncol, ap=[[1, 128], [256, nsl], [1, 128]])
                    ld(stage[:, 0:nsl, :], hank, ["Fdram"], ["stage"])
                    for c0 in range(0, nsl * 128, 512):
                        w = min(512, nsl * 128 - c0)
                        pt = psT[(c0 // 512) % 2]
                        st2 = stage[:, 0:nsl, :].rearrange("p s q -> p (s q)")
                        S.op("pe", lambda pt=pt, st2=st2, c0=c0, w=w: PE.matmul(
                            pt[:, 0:w], lhsT=J_f[:], rhs=st2[:, c0:c0 + w], start=True, stop=True),
                            ["J_f", "stage"], [("psT", (c0 // 512) % 2)])
                        s0 = c0 // 128
                        ns = w // 128
                        S.op("dve", lambda pt=pt, h=h, s0=s0, ns=ns, w=w: DVE.tensor_tensor(
                            out=slot[:, h, s0:s0 + ns, :], in0=pt[:, 0:w].rearrange("p (s q) -> p s q", q=128),
                            in1=mask[:, s0:s0 + ns, :], op=ALU.add),
                            [("psT", (c0 // 512) % 2), "mask"], ["slot"])

            ld(tabA[:], t5_table, [], ["tab"])
            ld(ohA[:], c_ohA, [], ["oh"])
            ld(mA[:], c_mA, [], ["mask"])
            build_slots([tabA[:, :]], [ohA], 4, NSLOT_A, FA, FA_t, mA, slotA)
            ld(tabB[:], b_rel[0:256, :].rearrange("(c p) h -> p c h", p=128), ["psF"], ["tab"])
            ld(ohB[:], c_ohB, ["psF"], ["oh"])
            ld(mB[:], c_mB, ["slot"], ["mask"])
            build_slots([tabB[:, 0, :], tabB[:, 1, :]], [ohB[:, 0, :], ohB[:, 1, :]], 8, NSLOT_B, FB, FB_t, mB, slotB)
            S.barrier()

        def attn_round(steps, nacc, vw, accs, acc_keys, pss_list, ptb, kq, vfn, slot_fn, tagbase, hook=None):
            bank_first = {}
            slots_of = {}

            def emit_qk(si):
                st = steps[si]
                pi = attn_round.ctr % len(pss_list)
                attn_round.ctr += 1
                slots_of[si] = pi
                pS = pss_list[pi]
                c0, c1 = st["a_lo"] * 128, (st["a_hi"] + 1) * 128
                nb = len(st["bias"])
                S.op("pe", lambda: PE.matmul(pS[:, c0:c1], lhsT=st["k"], rhs=st["q"], start=True, stop=(nb == 0),
                                             skip_group_check=True),
                     st["kq_reads"], [("pS", pi)], sig=(nb == 0))
                for bi, (a, sl_ap) in enumerate(st["bias"]):
                    S.op("pe", lambda a=a, sl_ap=sl_ap, bi=bi: PE.matmul(
                        pS[:, a * 128:(a + 1) * 128], lhsT=ident_b[:], rhs=sl_ap, start=False, stop=(bi == nb - 1),
                        skip_group_check=True), ["slots"], [("pS", pi)], sig=(bi == nb - 1))
                pt = ptb[pi]
                S.op("act", lambda: ACT.activation(out=pt[:, c0:c1], in_=pS[:, c0:c1], func=AF.Exp),
                     [("pS", pi)], [("pt", pi)])

            def emit_pv(si):
                st = steps[si]
                pi = slots_of[si]
                pt = ptb[pi]
                npv = len(st["pv"])
                for j_, (a, acc_ap, bank, stop) in enumerate(st["pv"]):
                    first = bank not in bank_first
                    bank_first[bank] = True
                    S.op("pe", lambda a=a, acc_ap=acc_ap, first=first, stop=stop: PE.matmul(
                        acc_ap, lhsT=pt[:, a * 128:(a + 1) * 128], rhs=st["v"], start=first, stop=stop,
                        skip_group_check=True), [("pt", pi)] + st["v_reads"], [("acc", bank)],
                        sig=(stop or j_ == npv - 1))

            n = len(steps)
            depth = 2
            for si in range(min(depth, n)):
                emit_qk(si)
            for si in range(n):
                if si + depth < n:
                    emit_qk(si + depth)
                emit_pv(si)
                if hook is not None and si == min(6, n - 1):
                    hook()

        attn_round.ctr = 0

        with ExitStack() as es:
            ktb = [[sb(es, "ktb%d_%d" % (i, hh_), [128, 20 * 128], BF16) for hh_ in range(2)] for i in range(2)]
            for i in range(2):
                for hh_ in range(2):
                    S.op("dve", lambda i=i, hh_=hh_: DVE.memset(ktb[i][hh_][(1 - hh_) * 64:(2 - hh_) * 64, :], 0.0),
                         [], [("ktb", i)])
            vbt = [sb(es, "vbt%d" % i, [128, 20, 132], BF16) for i in range(2)]
            ptb = [sb(es, "ptB%d" % i, [128, 512], BF16) for i in range(4)]
            obst = [sb(es, "obst%d" % i, [128, 4, 128], BF16) for i in range(2)]
            stB = [sb(es, "stB%d" % i, [128, 4, 66], F32) for i in range(2)]
            recb = sb(es, "recb", [128, 4], F32)
            defer = [None]
            pSs = [ps(es, "pSB%d" % i, [128, 512]) for i in range(4)]
            accBf = [ps(es, "accB%d" % i, [128, 512]) for i in range(2)]
            accB = [t_[:, 0:264].rearrange("p (a d) -> p a d", d=66) for t_ in accBf]
            ptr = ps(es, "ptrB", [128, 8, 128], BF16)[:, 0:4, :]
            rnd = 0
            def b_loads(it_):
                Gq_, pb_ = it_ // 4, it_ % 4
                bj = it_ % 2
                t0_ = 16 * Gq_ - 4
                tlo_ = max(t0_, 0)
                for hh_ in range(2):
                    ld(ktb[bj][hh_][hh_ * 64:(hh_ + 1) * 64, (tlo_ - t0_) * 128:20 * 128],
                       KT[4 + pb_, hh_ * 64:(hh_ + 1) * 64, tlo_ * 128:(t0_ + 20) * 128],
                       [("KT", 4 + pb_, g) for g in range(16)], [("ktb", bj)])
                ld(vbt[bj][:, tlo_ - t0_:20, :], VB[pb_, :, tlo_:t0_ + 20, :],
                   [("VB", g) for g in range(16)], [("vbt", bj)])

            b_loads(0)
            for Gq in range(4):
                for pb in range(4):
                    bi_ = (Gq * 4 + pb) % 2
                    t0 = 16 * Gq - 4
                    tlo = max(t0, 0)
                    if Gq * 4 + pb + 1 < 16:
                        b_loads(Gq * 4 + pb + 1)
                    if (Gq * 4 + pb) % 2 == 0:
                        precast(8 + (Gq * 4 + pb) // 2, [("ktb", bi_), ("vbt", bi_)])
                    for hh in range(2):
                        hb = 2 * pb + hh
                        prt = slice(hh * 64, (hh + 1) * 64)
                        acc = accB[rnd % 2]
                        steps = []
                        for s in range(20):
                            if t0 + s < 0:
                                continue
                            a_lo = max(0, -((7 - s) // 4))
                            a_hi = min(3, s // 4)
                            steps.append(dict(
                                a_lo=a_lo, a_hi=a_hi,
                                k=ktb[bi_][hh][:, s * 128:(s + 1) * 128],
                                q=QT[:, 4 + pb, Gq * 512 + a_lo * 128:Gq * 512 + (a_hi + 1) * 128],
                                kq_reads=[("ktb", bi_), "QT"],
                                bias=[(a, slotB[:, hb, s - 4 * a, :]) for a in range(a_lo, a_hi + 1)],
                                v=vbt[bi_][:, s, hh * 66:hh * 66 + 65], v_reads=[("vbt", bi_)],
                                pv=[(a, acc[:, a, 0:65], ("B", rnd % 2), (s - 4 * a == 7)) for a in range(a_lo, a_hi + 1)],
                            ))
                        hk = defer[0]
                        defer[0] = None
                        attn_round(steps, 4, 65, None, None, pSs, ptb, None, None, None, "B", hook=hk)
                        bank = ("acc", ("B", rnd % 2))
                        stg = stB[rnd % 2]
                        skey = ("stB", rnd % 2)
                        S.op("dve", lambda acc=acc, stg=stg: DVE.tensor_copy(out=stg[:], in_=acc), [bank], [skey])
                        S.op("dve", lambda stg=stg: DVE.reciprocal(out=recb[:], in_=stg[:, :, 64]), [skey], ["recb"])
                        S.op("dve", lambda stg=stg, hh=hh, ob_=obst[(Gq * 4 + pb) % 2]: DVE.tensor_tensor(
                            out=ob_[:, :, hh * 64:(hh + 1) * 64], in0=stg[:, :, 0:64],
                            in1=recb[:].unsqueeze(2).to_broadcast([128, 4, 64]), op=ALU.mult), [skey, "recb"], [("obst", (Gq * 4 + pb) % 2)])
                        rnd += 1

                    def fin(Gq=Gq, pb=pb, ob_=obst[(Gq * 4 + pb) % 2], okey=("obst", (Gq * 4 + pb) % 2)):
                        for a in range(4):
                            S.op("pe", lambda a=a: PE.transpose(out=ptr[:, a, :], in_=ob_[:, a, :], identity=ident_b[:]),
                                 [okey], ["ptr"], sig=(a == 3))
                        S.op("dve", lambda: DVE.tensor_copy(
                            out=OBT[:, pb, Gq * 512:(Gq + 1) * 512], in_=ptr.rearrange("p a q -> p (a q)")),
                            ["ptr"], ["OBT"])

                    defer[0] = fin
            if defer[0] is not None:
                defer[0]()
                defer[0] = None
            S.barrier()

        if DEBUG == "B":
            dbg = nc.dram_tensor("dbg", [128, 4, OWN], BF16, kind="ExternalOutput").ap()
            ld(dbg, OBT[:], ["OBT"], ["dbg"])
            S.wait_all("sp", ["dbg"])
            return nc

        with ExitStack() as es:
            k1t = [sb(es, "k1t%d" % i, [128, SEQ], BF16) for i in range(2)]
            k2t = [sb(es, "k2t%d" % i, [128, SEQ], BF16) for i in range(2)]
            for i in range(2):
                S.op("dve", lambda i=i: DVE.memset(k1t[i][(1 - i) * 64:(2 - i) * 64, :], 0.0), [], ["k1t"])
                S.op("dve", lambda i=i: DVE.memset(k2t[i][(1 - i) * 64:(2 - i) * 64, :], 0.0), [], ["k2t"])
            vat = sb(es, "vat", [128, 64, 130], BF16)
            ptb = [sb(es, "ptA%d" % i, [128, 512], BF16) for i in range(4)]
            oast = [sb(es, "oast%d" % i, [128, 4, 128], BF16) for i in range(2)]
            ostA = [sb(es, "ostA%d" % i, [128, 8, 130], F32) for i in range(2)]
            rec = sb(es, "recA", [128, 8], F32)
            t1 = sb(es, "t1A", [128, 4, 128], F32)
            dd = sb(es, "ddA", [128, 4, 128], F32)
            ssA = sb(es, "ssA", [128, 8], F32)
            deferA = [None]
            rndA = [0]
            pSs = [ps(es, "pSA%d" % i, [128, 512]) for i in range(4)]
            accA = [ps(es, "accA%d" % i, [128, 512]) for i in range(3)]
            ptr = ps(es, "ptrA", [128, 8, 128], BF16)[:, 0:4, :]

            def acc_ap(s, a):
                idx = s * 4 + a
                return accA[idx // 3][:, (idx % 3) * 130:(idx % 3) * 130 + 129], ("A", idx // 3)

            for hp in range(2):
                for i in range(2):
                    ld(k1t[i][i * 64:(i + 1) * 64, :], KT[hp, i * 64:(i + 1) * 64, :], [("KT", hp, g) for g in range(16)], ["k1t"])
                    ld(k2t[i][i * 64:(i + 1) * 64, :], KT[2 + hp, i * 64:(i + 1) * 64, :], [("KT", 2 + hp, g) for g in range(16)], ["k2t"])
                for hh in range(2):
                    h = 2 * hp + hh
                    prt = slice(hh * 64, (hh + 1) * 64)
                    ld(vat[:], VA[h], [("VA", g) for g in range(16)], ["vat"])
                    for Gq in range(4):
                        precast(16 + (hp * 2 + hh) * 4 + Gq, ["vat", "OAT"])
                        steps = []
                        for kt in range(16 * Gq + 16):
                            a_lo = max(0, -((16 * Gq + 3 - kt) // 4))
                            for s in range(2):
                                ksrc = (k1t if s == 0 else k2t)[hh]
                                bias = []
                                for a in range(a_lo, 4):
                                    r = kt - 16 * Gq - 4 * a
                                    if -1 <= r <= 3:
                                        bias.append((a, slotA[:, h, r + 1, :]))
                                pv = []
                                for a in range(a_lo, 4):
                                    ap_, bank = acc_ap(s, a)
                                    pv.append((a, ap_, bank, kt == 16 * Gq + 4 * a + 3))
                                steps.append(dict(
                                    a_lo=a_lo, a_hi=3,
                                    k=ksrc[:, kt * 128:(kt + 1) * 128],
                                    q=QT[:, s * 2 + hp, Gq * 512 + a_lo * 128:(Gq + 1) * 512],
                                    kq_reads=["k1t" if s == 0 else "k2t", "QT"],
                                    bias=bias, v=vat[:, kt, 0:129], v_reads=["vat"], pv=pv))
                        hk = deferA[0]
                        deferA[0] = None
                        attn_round(steps, 8, 129, None, None, pSs, ptb, None, None, None, "A", hook=hk)
                        p_ = rndA[0] % 2
                        rndA[0] += 1
                        ost = ostA[p_]
                        okey = ("ostA", p_)
                        ofl = ost[:].rearrange("p a d -> p (a d)")
                        for i in range(3):
                            ncol = 390 if i < 2 else 260
                            S.op("dve", lambda i=i, ncol=ncol: DVE.tensor_copy(out=ofl[:, i * 390:i * 390 + ncol], in_=accA[i][:, 0:ncol]),
                                 [("acc", ("A", i))], [okey])
                        S.op("dve", lambda: DVE.reciprocal(out=rec[:], in_=ost[:, :, 128]), [okey], ["recA"])
                        S.op("dve", lambda: DVE.tensor_scalar(out=rec[:, 4:8], in0=rec[:, 4:8], scalar1=neglam[:, 0:1],
                                                              scalar2=None, op0=ALU.mult), ["recA"], ["recA"])
                        S.op("dve", lambda: DVE.tensor_tensor(out=t1[:], in0=ost[:, 0:4, 0:128],
                                                              in1=rec[:, 0:4].unsqueeze(2).to_broadcast([128, 4, 128]), op=ALU.mult),
                             [okey, "recA"], ["t1A"])
                        S.op("dve", lambda: DVE.tensor_tensor(out=dd[:], in0=ost[:, 4:8, 0:128],
                                                              in1=rec[:, 4:8].unsqueeze(2).to_broadcast([128, 4, 128]), op=ALU.mult),
                             [okey, "recA"], ["ddA"])
                        S.op("dve", lambda: DVE.tensor_tensor(out=dd[:], in0=dd[:], in1=t1[:], op=ALU.add), ["ddA", "t1A"], ["ddA"])
                        S.op("dve", lambda: DVE.tensor_tensor(out=t1[:], in0=dd[:], in1=dd[:], op=ALU.mult), ["ddA"], ["t1A"])
                        S.op("dve", lambda: DVE.tensor_reduce(out=ssA[:, 0:4], in_=t1[:], axis=AX.X, op=ALU.add), ["t1A"], ["ssA0"])
                        S.op("dve", lambda: DVE.tensor_scalar(out=ssA[:, 0:4], in0=ssA[:, 0:4], scalar1=1.0 / 128, scalar2=EPS,
                                                              op0=ALU.mult, op1=ALU.add), ["ssA0"], ["ssA0"])
                        S.op("act", lambda: ACT.activation(out=ssA[:, 4:8], in_=ssA[:, 0:4], func=AF.Ln), ["ssA0"], ["ssA1"])
                        S.op("act", lambda: ACT.activation(out=ssA[:, 4:8], in_=ssA[:, 4:8], func=AF.Exp, scale=-0.5), ["ssA1"], ["ssA1"])
                        oa_ = oast[p_]
                        S.op("dve", lambda: DVE.tensor_tensor(out=dd[:], in0=dd[:], in1=ssA[:, 4:8].unsqueeze(2).to_broadcast([128, 4, 128]),
                                                              op=ALU.mult), ["ddA", "ssA1"], ["ddA"])
                        S.op("dve", lambda oa_=oa_: DVE.tensor_tensor(out=oa_[:], in0=dd[:], in1=gsub[:].unsqueeze(1).to_broadcast([128, 4, 128]),
                                                                      op=ALU.mult), ["ddA"], [("oast", p_)])

                        def finA(Gq=Gq, h=h, oa_=oa_, p_=p_):
                            for a in range(4):
                                S.op("pe", lambda a=a: PE.transpose(out=ptr[:, a, :], in_=oa_[:, a, :], identity=ident_b[:]),
                                     [("oast", p_)], ["ptr"], sig=(a == 3))
                            S.op("dve", lambda: DVE.tensor_copy(
                                out=OAT[:, h, Gq * 512:(Gq + 1) * 512], in_=ptr.rearrange("p a q -> p (a q)")),
                                ["ptr"], ["OAT"])

                        deferA[0] = finA
            if deferA[0] is not None:
                deferA[0]()
                deferA[0] = None
            S.barrier()

        if DEBUG == "A":
            dbg = nc.dram_tensor("dbg", [128, 4, OWN], BF16, kind="ExternalOutput").ap()
            ld(dbg, OAT[:], ["OAT"], ["dbg"])
            S.wait_all("sp", ["dbg"])
            return nc

        qs.close()
        with ExitStack() as es:
            xmt = [sb(es, "xmt%d" % i, [128, D], F32) for i in range(2)]
            wg = sb(es, "wg", [128, 8, 2048], BF16)
            wA = sb(es, "wA", [128, 4, D], BF16)
            wB = sb(es, "wB", [128, 4, D], BF16)
            wO = sb(es, "wO", [128, 8, D], BF16)
            xTb = [sb(es, "xTm%d" % i, [128, 8, 512], BF16) for i in range(2)]
            xsqm = [sb(es, "xsqm%d" % i, [128, 8, 512], BF16) for i in range(2)]
            xnb = [sb(es, "xnm%d" % i, [128, 8, 512], BF16) for i in range(2)]
            rtmpm = [sb(es, "rtmpm%d" % i, [128, 512], F32) for i in range(2)]
            rstdm = [sb(es, "rstdm%d" % i, [128, 512], F32) for i in range(2)]
            sga = [sb(es, "sga%d" % i, [128, 512], F32) for i in range(2)]
            sgb = [sb(es, "sgb%d" % i, [128, 512], F32) for i in range(2)]
            m1 = [sb(es, "m1%d" % i, [128, 512], F32) for i in range(2)]
            m2 = [sb(es, "m2%d" % i, [128, 512], F32) for i in range(2)]
            mixT = sb(es, "mixT", [128, 8, 512], BF16)
            pss = ps(es, "pssm", [128, 512])
            psg = [ps(es, "psg%d" % i, [128, 512]) for i in range(2)]
            psy = [ps(es, "psy%d" % i, [128, 512]) for i in range(2)]
            psx = [ps(es, "psx%d" % i, [128, 512]) for i in range(2)]
            def norm_m(Gq):
                p = Gq % 2
                return norm_pieces((xTb[p], xsqm[p], xnb[p], rtmpm[p], rstdm[p], pss), xT_own[:, Gq * 512:(Gq + 1) * 512], ("m", p))

            pcs0, xn_first = norm_m(0)
            pcs0[0]()
            wevs = []
            for hf in range(4):
                wevs.append(ld(wg[:, :, hf * 512:(hf + 1) * 512],
                               w_in[:, 3072 + hf * 512:3072 + (hf + 1) * 512].rearrange("(c p) n -> p c n", p=128), [], [], cast=True))
            for hf in range(2):
                wevs.append(ld(wO[:, :, hf * 512:(hf + 1) * 512],
                               w_out[:, hf * 512:(hf + 1) * 512].rearrange("(c p) n -> p c n", p=128), [], [], cast=True))
                wevs.append(ld(wA[:, :, hf * 512:(hf + 1) * 512],
                               w_ba[:, hf * 512:(hf + 1) * 512].rearrange("(c p) n -> p c n", p=128), [], [], cast=True))
                wevs.append(ld(wB[:, :, hf * 512:(hf + 1) * 512],
                               w_bb[:, hf * 512:(hf + 1) * 512].rearrange("(c p) n -> p c n", p=128), [], [], cast=True))
            S.wait_evs("pe", wevs)
            it = 0
            for p_ in pcs0[1:]:
                p_()
            xn_nextm = xn_first
            for Gq in range(4):
                xn = xn_nextm
                pcs = []
                if Gq + 1 < 4:
                    pcs, xn_nextm = norm_m(Gq + 1)
                tg = ("m", Gq % 2)
                for cc in range(8):
                    for _ in range(2 if cc >= 2 else 1):
                        if pcs:
                            pcs.pop(0)()
                    p = it % 2
                    it += 1
                    for br in range(2):
                        pg = psg[br]
                        py = psy[br]
                        for c in range(8):
                            S.op("pe", lambda c=c, pg=pg, br=br, cc=cc: PE.matmul(
                                pg[:], lhsT=wg[:, c, br * 1024 + cc * 128:br * 1024 + (cc + 1) * 128], rhs=xn[:, c, :],
                                start=(c == 0), stop=(c == 7)), [("xn", tg, c)], [("psg", br)], sig=(c == 7))
                        sg_t = (sga if br == 0 else sgb)[p]
                        S.op("act", lambda sg_t=sg_t, pg=pg: ACT.activation(out=sg_t[:], in_=pg[:], func=AF.Sigmoid),
                             [("psg", br)], [("sg", br, p)])
                        wbr = wA if br == 0 else wB
                        obr = OAT if br == 0 else OBT
                        for c in range(4):
                            S.op("pe", lambda c=c, py=py, wbr=wbr, obr=obr, cc=cc, Gq=Gq: PE.matmul(
                                py[:], lhsT=wbr[:, c, cc * 128:(cc + 1) * 128], rhs=obr[:, c, Gq * 512:(Gq + 1) * 512],
                                start=(c == 0), stop=(c == 3)), [], [("psy", br)], sig=(c == 3))
                        mm = (m1 if br == 0 else m2)[p]
                        S.op("dve", lambda mm=mm, py=py, sg_t=sg_t: DVE.tensor_tensor(out=mm[:], in0=py[:], in1=sg_t[:], op=ALU.mult),
                             [("psy", br), ("sg", br, p)], [("mm", br, p)])
                    S.op("pool", lambda cc=cc, p=p: POOL.tensor_tensor(out=mixT[:, cc, :], in0=m1[p][:], in1=m2[p][:], op=ALU.add),
                         [("mm", 0, p), ("mm", 1, p)], [("mixT", cc)])
                while pcs:
                    pcs.pop(0)()
                for tt in range(4):
                    t = Gq * 4 + tt
                    xm_ = xmt[t % 2]
                    ld(xm_[:], x_own[t * 128:(t + 1) * 128, :], [], [("xmt", t % 2)])
                    for half in range(2):
                        px = psx[half]
                        for cc in range(8):
                            S.op("pe", lambda cc=cc, px=px, tt=tt, half=half: PE.matmul(
                                px[:], lhsT=mixT[:, cc, tt * 128:(tt + 1) * 128], rhs=wO[:, cc, half * 512:(half + 1) * 512],
                                start=(cc == 0), stop=(cc == 7)), [("mixT", cc)], [("psx", half)], sig=(cc == 7))
                        S.op("dve", lambda px=px, xm_=xm_, half=half: DVE.tensor_tensor(
                            out=xm_[:, half * 512:(half + 1) * 512], in0=px[:], in1=xm_[:, half * 512:(half + 1) * 512],
                            op=ALU.add), [("psx", half), ("xmt", t % 2)], [("xmt", t % 2)])
                    ld(XM[t * 128:(t + 1) * 128, :], xm_[:], [("xmt", t % 2)], [("XM", t)])
            S.barrier()
        mid.close()

        if DEBUG == "M":
            dbg = nc.dram_tensor("dbg", [OWN, D], F32, kind="ExternalOutput").ap()
            with ExitStack() as es:
                dt_ = sb(es, "dbgt", [128, D], F32)
                for t in range(16):
                    ld(dt_[:], XM[t * 128:(t + 1) * 128, :], [("XM", t)], ["dbgt"])
                    ld(dbg[t * 128:(t + 1) * 128, :], dt_[:], ["dbgt"], [("dbg", t)])
                S.wait_all("sp", [("dbg", t) for t in range(16)])
            return nc

        with ExitStack() as es:
            g2 = sb(es, "g2", [128, D], F32)
            wr = sb(es, "wr", [128, 8, 36], F32)
            brt = sb(es, "brt", [128, 36], F32)
            U_f = sb(es, "U_f", [128, 128], F32)
            U_b = sb(es, "U_b", [128, 128], BF16)
            iotaC = sb(es, "iotaC", [128, NEXP], F32)
            iota_i = sb(es, "iota_i", [128, NEXP], mybir.dt.int32)
            idx_all = sb(es, "idx_all", [128, 16, 2], U32)
            comb_all = sb(es, "comb_all", [128, 16, 2], F32)
            junk = sb(es, "junk", [128, D], F32)
            hn = sb(es, "hn", [128, D], F32)
            hnT = sb(es, "hnT", [128, 8, 128], F32)
            sm = sb(es, "sm", [128, 64], F32)
            wgt = [sb(es, "wgt%d" % i, [128, 8, 512], BF16) for i in range(3)]
            wup = [sb(es, "wup%d" % i, [128, 8, 512], BF16) for i in range(3)]
            wdn = [sb(es, "wdn%d" % i, [128, 4, D], BF16) for i in range(3)]
            Xe = [sb(es, "Xe%d" % i, [128, 2, D], BF16) for i in range(2)]
            XeT = [sb(es, "XeT%d" % i, [128, 8, CAP], BF16) for i in range(2)]
            sgl = [sb(es, "sgl%d" % i, [128, CAP], F32) for i in range(2)]
            hT = [sb(es, "hT%d" % i, [128, 4, CAP], BF16) for i in range(2)]
            yst = [sb(es, "yst%d" % i, [128, D], BF16) for i in range(4)]
            gb1 = [sb(es, "gb1%d" % i, [128, D], BF16) for i in range(2)]
            gb2 = [sb(es, "gb2%d" % i, [128, D], BF16) for i in range(2)]
            ptf = [ps(es, "ptf%d" % i, [128, 4, 128]) for i in range(2)]
            psl = ps(es, "psl", [128, 512])
            ptb_ = ps(es, "ptbE", [128, 8, 128], BF16)
            pg_ = ps(es, "pgE", [128, 512])
            pu_ = ps(es, "puE", [128, 512])
            py_ = [ps(es, "pyE%d" % i, [128, 512]) for i in range(2)]

            bc_reg = POOL.alloc_register()
            POOL.reg_mov(bc_reg, NEXP * CAP - 1)
            ld(g2[:], norm2_g.partition_broadcast(128).rearrange("p o d -> p (o d)"), [], ["g2"])
            ld(wr[:, :, 0:4], w_rg.rearrange("(c p) n -> p c n", p=128), [], ["wr"])
            ld(wr[:, :, 4:36], w_re.rearrange("(c p) n -> p c n", p=128), [], ["wr"])
            ld(brt[:, 0:4], b_rg.partition_broadcast(128).rearrange("p o d -> p (o d)"), [], ["brt"])
            ld(brt[:, 4:36], b_re.partition_broadcast(128).rearrange("p o d -> p (o d)"), [], ["brt"])
            ld(U_f[:], c_U, [], ["U_f"])
            S.op("dve", lambda: DVE.tensor_copy(out=U_b[:], in_=U_f[:]), ["U_f"], ["U_b"])
            S.op("pool", lambda: POOL.iota(iota_i[:], pattern=[[CAP, NEXP]], base=0, channel_multiplier=0), [], ["iota_i"])
            S.op("dve", lambda: DVE.tensor_copy(out=iotaC[:], in_=iota_i[:]), ["iota_i"], ["iotaC"])

            def wfetch(e):
                b = e % 3
                ld(wgt[b][:].rearrange("p c n -> p (c n)"), WG16[e], [("W16", e, 0)], [("wgt", b)])
                ld(wup[b][:].rearrange("p c n -> p (c n)"), WU16[e], [("W16", e, 1)], [("wup", b)])
                ld(wdn[b][:].rearrange("p c n -> p (c n)"), WD16[e], [("W16", e, 2, 0), ("W16", e, 2, 1)], [("wdn", b)])


            xme = [sb(es, "xme%d" % i, [128, D], F32) for i in range(2)]
            hnb_all = sb(es, "hnb_all", [128, 16, D], BF16)
            lgg = sb(es, "lgg", [128, 16, 4], F32)
            lge = sb(es, "lge", [128, 16, NEXP], F32)
            junk2 = [junk, sb(es, "junk_b", [128, D], F32)]
            hn2 = [hn, sb(es, "hn_b", [128, D], F32)]
            hnT2 = [hnT, sb(es, "hnT_b", [128, 8, 128], F32)]
            def r_front(t):
                q_ = t % 2
                jk, hn_ = junk2[q_], hn2[q_]
                o = 2 * q_
                ld(xme[q_][:], XM[t * 128:(t + 1) * 128, :], [("XM", t)], [("xme", q_)])
                if t in (3, 7, 11):
                    wfetch(t // 4)
                xm = xme[q_][:]
                S.op("dve", lambda: DVE.tensor_tensor(out=jk[:], in0=xm, in1=xm, op=ALU.mult), [("xme", q_)], [("junk", q_)])
                S.op("dve", lambda: DVE.tensor_reduce(out=sm[:, o:o + 1], in_=jk[:], axis=AX.X, op=ALU.add),
                     [("junk", q_)], [("sm0", q_)])
                S.op("dve", lambda: DVE.tensor_scalar(out=sm[:, o:o + 1], in0=sm[:, o:o + 1], scalar1=1.0 / D, scalar2=EPS,
                                                      op0=ALU.mult, op1=ALU.add), [("sm0", q_)], [("sm0", q_)])
                S.op("act", lambda: ACT.activation(out=sm[:, o + 1:o + 2], in_=sm[:, o:o + 1], func=AF.Ln), [("sm0", q_)], [("sm1", q_)])
                S.op("act", lambda: ACT.activation(out=sm[:, o + 1:o + 2], in_=sm[:, o + 1:o + 2], func=AF.Exp, scale=-0.5),
                     [("sm1", q_)], [("sm1", q_)])
                S.op("dve", lambda: DVE.scalar_tensor_tensor(out=hn_[:], in0=xm, scalar=sm[:, o + 1:o + 2], in1=g2[:],
                                                             op0=ALU.mult, op1=ALU.mult),
                     [("xme", q_), ("sm1", q_), "g2"], [("hn", q_)])
                S.op("act", lambda: ACT.activation(out=hnb_all[:, t, :], in_=hn_[:], func=AF.Copy), [("hn", q_)], [("hnb", t)])

            def r_back(t):
                q_ = t % 2
                hn_, hnT_ = hn2[q_], hnT2[q_]
                po = q_ * 64
                for half in range(2):
                    pf = ptf[half]
                    for c4 in range(4):
                        c = half * 4 + c4
                        S.op("pe", lambda pf=pf, c4=c4, c=c: PE.transpose(out=pf[:, c4, :], in_=hn_[:, c * 128:(c + 1) * 128],
                                                                           identity=ident_f[:]), [("hn", q_)], [("ptf", half)], sig=(c4 == 3))
                    S.op("dve", lambda pf=pf, half=half: DVE.tensor_copy(out=hnT_[:, half * 4:(half + 1) * 4, :], in_=pf[:]),
                         [("ptf", half)], [("hnT", q_, half)])
                for c in range(8):
                    S.op("pe", lambda c=c: PE.matmul(psl[:, po:po + 36], lhsT=hnT_[:, c, :], rhs=wr[:, c, :],
                                                     start=(c == 0), stop=(c == 7)),
                         [("hnT", q_, c // 4), "wr"], [("psl", q_)], sig=(c == 7))
                S.op("dve", lambda: DVE.tensor_tensor(out=lgg[:, t, :], in0=psl[:, po:po + 4], in1=brt[:, 0:4], op=ALU.add),
                     [("psl", q_), "brt"], ["lgg"])
                S.op("dve", lambda: DVE.tensor_tensor(out=lge[:, t, :], in0=psl[:, po + 4:po + 36], in1=brt[:, 4:36], op=ALU.add),
                     [("psl", q_), "brt"], ["lge"])

            r_front(0)
            for t in range(16):
                if t + 1 < 16:
                    r_front(t + 1)
                r_back(t)

            gm = sb(es, "gm", [128, 16], F32)
            gohb = sb(es, "gohb", [128, 16, 4], F32)
            shb = sb(es, "shb", [128, 16, 4], F32)
            egb = sb(es, "egb", [128, 16, 4], F32)
            sgb_ = sb(es, "sgb_", [128, 16], F32)
            pgb = sb(es, "pgb", [128, 16], F32)
            penb = sb(es, "penb", [128, 16, 4], F32)
            leb = sb(es, "leb", [128, 16, NEXP], F32)
            m8b = sb(es, "m8b", [128, 16, 8], F32)
            ohb = [sb(es, "ohb%d" % k, [128, 16, NEXP], F32) for k in range(2)]
            d21 = sb(es, "d21", [128, 16], F32)
            e21 = sb(es, "e21", [128, 16], F32)
            w1b = sb(es, "w1b", [128, 16], F32)
            a12b = sb(es, "a12b", [128, 16, NEXP], BF16)
            carx = sb(es, "carx", [128, 16, NEXP], F32)
            Rb = sb(es, "Rb", [128, 16, NEXP], F32)
            ovb = sb(es, "ovb", [128, 16, NEXP], F32)
            slb = sb(es, "slb", [128, 16, NEXP], F32)
            prb = sb(es, "prb", [128, 16, NEXP], F32)
            sl2b = sb(es, "sl2b", [128, 2, 16], F32)
            vlb = sb(es, "vlb", [128, 2, 16], F32)

            def bc3(ap2, n):
                return ap2.unsqueeze(2).to_broadcast([128, 16, n])

            S.op("dve", lambda: DVE.tensor_reduce(out=gm[:], in_=lgg[:], axis=AX.X, op=ALU.max), ["lgg"], ["gm"])
            S.op("dve", lambda: DVE.tensor_tensor(out=gohb[:], in0=lgg[:], in1=bc3(gm[:], 4), op=ALU.is_equal), ["lgg", "gm"], ["gohb"])
            S.op("dve", lambda: DVE.tensor_tensor(out=shb[:], in0=lgg[:], in1=bc3(gm[:], 4), op=ALU.subtract), ["lgg", "gm"], ["shb"])
            S.op("act", lambda: ACT.activation(out=egb[:], in_=shb[:], func=AF.Exp), ["shb"], ["egb"])
            S.op("dve", lambda: DVE.tensor_reduce(out=sgb_[:], in_=egb[:], axis=AX.X, op=ALU.add), ["egb"], ["sgb_"])
            S.op("dve", lambda: DVE.reciprocal(out=pgb[:], in_=sgb_[:]), ["sgb_"], ["pgb"])
            S.op("dve", lambda: DVE.tensor_scalar(out=penb[:], in0=gohb[:], scalar1=1e9, scalar2=-1e9, op0=ALU.mult, op1=ALU.add),
                 ["gohb"], ["penb"])
            S.op("dve", lambda: DVE.tensor_tensor(
                out=leb[:].rearrange("p t (g e) -> p (t g) e", e=8), in0=lge[:].rearrange("p t (g e) -> p (t g) e", e=8),
                in1=penb[:].rearrange("p t g -> p (t g)").unsqueeze(2).to_broadcast([128, 64, 8]), op=ALU.add),
                ["lge", "penb"], ["leb"])
            for t in range(16):
                S.op("dve", lambda t=t: DVE.max(out=m8b[:, t, :], in_=leb[:, t, :]), ["leb"], ["m8b"])
            for k in range(2):
                S.op("dve", lambda k=k: DVE.tensor_tensor(out=ohb[k][:], in0=leb[:], in1=m8b[:, :, k:k + 1].to_broadcast([128, 16, NEXP]),
                                                          op=ALU.is_equal), ["leb", "m8b"], [("ohb", k)])
            S.op("dve", lambda: DVE.tensor_tensor(out=d21[:], in0=m8b[:, :, 1], in1=m8b[:, :, 0], op=ALU.subtract), ["m8b"], ["d21"])
            S.op("act", lambda: ACT.activation(out=e21[:], in_=d21[:], func=AF.Exp), ["d21"], ["e21"])
            S.op("dve", lambda: DVE.tensor_scalar(out=w1b[:], in0=e21[:], scalar1=1.0, scalar2=None, op0=ALU.add), ["e21"], ["w1b"])
            S.op("dve", lambda: DVE.reciprocal(out=w1b[:], in_=w1b[:]), ["w1b"], ["w1b"])
            S.op("dve", lambda: DVE.tensor_tensor(out=comb_all[:, :, 0], in0=w1b[:], in1=pgb[:], op=ALU.mult), ["w1b", "pgb"], ["comb0"])
            S.op("dve", lambda: DVE.tensor_tensor(out=comb_all[:, :, 1], in0=comb_all[:, :, 0], in1=e21[:], op=ALU.mult),
                 ["comb0", "e21"], ["comb1"])
            S.op("dve", lambda: DVE.tensor_tensor(out=a12b[:], in0=ohb[0][:], in1=ohb[1][:], op=ALU.add), [("ohb", 0), ("ohb", 1)], ["a12b"])
            for t in range(16):
                S.op("pe", lambda t=t: PE.matmul(pg_[:, t * 32:(t + 1) * 32], lhsT=U_b[:], rhs=a12b[:, t, :], start=True, stop=True,
                                                 skip_group_check=True), ["a12b", "U_b"], ["pgE"], sig=(t == 15))
            for t in range(16):
                S.op("pe", lambda t=t: PE.matmul(pu_[:, t * 32:(t + 1) * 32], lhsT=ones_b[:], rhs=a12b[:, t, :], start=True, stop=True,
                                                 skip_group_check=True), ["a12b"], ["puE"], sig=(t == 15))
            S.op("dve", lambda: DVE.memset(carx[:, 0, :], 0.0), [], ["carx"])
            for t in range(1, 16):
                S.op("dve", lambda t=t: DVE.tensor_tensor(out=carx[:, t, :], in0=pu_[:, (t - 1) * 32:t * 32], in1=carx[:, t - 1, :],
                                                          op=ALU.add), ["puE", "carx"], ["carx"])
            S.op("dve", lambda: DVE.tensor_tensor(out=Rb[:], in0=pg_[:, 0:512].rearrange("p (t e) -> p t e", e=NEXP), in1=carx[:],
                                                  op=ALU.add), ["pgE", "carx"], ["Rb"])
            S.op("dve", lambda: DVE.tensor_scalar(out=ovb[:], in0=Rb[:], scalar1=float(CAP), scalar2=1e6, op0=ALU.is_ge, op1=ALU.mult),
                 ["Rb"], ["ovb"])
            S.op("dve", lambda: DVE.tensor_tensor(out=slb[:], in0=Rb[:], in1=iotaC[:].unsqueeze(1).to_broadcast([128, 16, NEXP]),
                                                  op=ALU.add), ["Rb", "iotaC"], ["slb"])
            S.op("dve", lambda: DVE.tensor_tensor(out=slb[:], in0=slb[:], in1=ovb[:], op=ALU.add), ["slb", "ovb"], ["slb"])
            for k in range(2):
                S.op("dve", lambda k=k: DVE.tensor_tensor(out=prb[:], in0=ohb[k][:], in1=slb[:], op=ALU.mult), [("ohb", k), "slb"], ["prb"])
                S.op("dve", lambda k=k: DVE.tensor_reduce(out=sl2b[:, k, :], in_=prb[:], axis=AX.X, op=ALU.add), ["prb"], [("sl2b", k)])
                S.op("dve", lambda k=k: DVE.tensor_scalar(out=vlb[:, k, :], in0=sl2b[:, k, :], scalar1=5e5, scalar2=None, op0=ALU.is_lt),
                     [("sl2b", k)], [("vlb", k)])
                S.op("dve", lambda k=k: DVE.tensor_tensor(out=comb_all[:, :, k], in0=comb_all[:, :, k], in1=vlb[:, k, :], op=ALU.mult),
                     [("vlb", k), "comb0", "comb1"], ["comb%d" % k])
                S.op("dve", lambda k=k: DVE.tensor_copy(out=idx_all[:, :, k], in_=sl2b[:, k, :]), [("sl2b", k)], [("idx", k)])
            for t in range(16):
                for k in range(2):
                    S.op("pool", lambda t=t, k=k: POOL.indirect_dma_start(
                        out=XS, out_offset=bass.IndirectOffsetOnAxis(ap=idx_all[:, t, k:k + 1], axis=0),
                        in_=hnb_all[:, t, :], in_offset=None, bounds_check=bc_reg, oob_is_err=False),
                        [("hnb", t), ("idx", 0), ("idx", 1)], [("XS", t, k)], dma=True)

            xs_keys = [("XS", t, k) for t in range(16) for k in range(2)]

            def xload(e):
                b = e % 2
                ld(Xe[b][:], XS[e * CAP:(e + 1) * CAP, :].rearrange("(i p) d -> p i d", p=128), xs_keys if e == 0 else [], [("Xe", b)])

            xload(0)
            for e in range(NEXP):
                b = e % 2
                w3 = e % 3
                if e + 1 < NEXP:
                    xload(e + 1)
                for i in range(2):
                    for c in range(8):
                        S.op("pe", lambda i=i, c=c, b=b: PE.transpose(out=ptb_[:, c, :], in_=Xe[b][:, i, c * 128:(c + 1) * 128],
                                                                      identity=ident_b[:]), [("Xe", b)], ["ptbE"], sig=(c == 7))
                    S.op("act", lambda i=i, b=b: ACT.activation(out=XeT[b][:, :, i * 128:(i + 1) * 128], in_=ptb_[:], func=AF.Copy),
                         ["ptbE"], [("XeT", b)])
                for fc in range(4):
                    for c in range(8):
                        S.op("pe", lambda c=c, fc=fc, b=b, w3=w3: PE.matmul(pg_[:, 0:CAP], lhsT=wgt[w3][:, c, fc * 128:(fc + 1) * 128],
                                                                              rhs=XeT[b][:, c, :], start=(c == 0), stop=(c == 7)),
                             [("wgt", w3), ("XeT", b)], ["pgE"], sig=(c == 7))
                    for c in range(8):
                        S.op("pe", lambda c=c, fc=fc, b=b, w3=w3: PE.matmul(pu_[:, 0:CAP], lhsT=wup[w3][:, c, fc * 128:(fc + 1) * 128],
                                                                              rhs=XeT[b][:, c, :], start=(c == 0), stop=(c == 7)),
                             [("wup", w3), ("XeT", b)], ["puE"], sig=(c == 7))
                    sgt = sgl[fc % 2]
                    S.op("act", lambda sgt=sgt: ACT.activation(out=sgt[:], in_=pg_[:, 0:CAP], func=AF.Silu), ["pgE"], [("sgl", fc % 2)])
                    S.op("dve", lambda sgt=sgt, fc=fc, b=b: DVE.tensor_tensor(out=hT[b][:, fc, :], in0=pu_[:, 0:CAP], in1=sgt[:], op=ALU.mult),
                         ["puE", ("sgl", fc % 2)], [("hT", b)])
                for i in range(2):
                    ys = yst[(e * 2 + i) % 4]
                    yk = ("yst", (e * 2 + i) % 4)
                    for half in range(2):
                        py = py_[half]
                        for fc in range(4):
                            S.op("pe", lambda fc=fc, i=i, half=half, py=py, b=b, w3=w3: PE.matmul(
                                py[:], lhsT=hT[b][:, fc, i * 128:(i + 1) * 128], rhs=wdn[w3][:, fc, half * 512:(half + 1) * 512],
                                start=(fc == 0), stop=(fc == 3)), [("hT", b), ("wdn", w3)], [("pyE", half)], sig=(fc == 3))
                        if half == 0:
                            S.op("act", lambda ys=ys, py=py: ACT.activation(out=ys[:, 0:512], in_=py[:], func=AF.Copy),
                                 [("pyE", half)], [yk])
                        else:
                            S.op("dve", lambda ys=ys, py=py: DVE.tensor_copy(out=ys[:, 512:1024], in_=py[:]), [("pyE", half)], [yk])
                    ld(YS[e * CAP + i * 128:e * CAP + (i + 1) * 128, :], ys[:], [yk], [("YS", e, i)])
                if e + 3 < NEXP:
                    wfetch(e + 3)

            ys_keys = [("YS", e, i) for e in range(NEXP) for i in range(2)]
            o1 = junk2
            for b in range(2):
                S.op("pool", lambda b=b: POOL.memset(gb1[b][:], 0.0), [], [("gb1", b)])
                S.op("pool", lambda b=b: POOL.memset(gb2[b][:], 0.0), [], [("gb2", b)])
            for t in range(16):
                b = t % 2
                ld(xme[b][:], XM[t * 128:(t + 1) * 128, :], [("XM", t)], [("xme", b)])
                for k, gbuf in enumerate((gb1, gb2)):
                    S.op("pool", lambda t=t, k=k, gbuf=gbuf, b=b: POOL.indirect_dma_start(
                        out=gbuf[b][:, :], out_offset=None, in_=YS[:, :],
                        in_offset=bass.IndirectOffsetOnAxis(ap=idx_all[:, t, k:k + 1], axis=0),
                        bounds_check=bc_reg, oob_is_err=False),
                        (ys_keys if t == 0 and k == 0 else []) + [("idx", 0), ("idx", 1)], [("gb%d" % (k + 1), b)], dma=True)
                S.op("dve", lambda t=t, b=b: DVE.scalar_tensor_tensor(out=o1[b][:], in0=gb1[b][:], scalar=comb_all[:, t, 0:1],
                                                                      in1=xme[b][:], op0=ALU.mult, op1=ALU.add),
                     [("gb1", b), "comb0", ("xme", b)], [("o1", b)])
                S.op("dve", lambda t=t, b=b: DVE.scalar_tensor_tensor(out=o1[b][:], in0=gb2[b][:], scalar=comb_all[:, t, 1:2],
                                                                      in1=o1[b][:], op0=ALU.mult, op1=ALU.add),
                     [("gb2", b), "comb1", ("o1", b)], [("o1", b)])
                ld(out_own[t * 128:(t + 1) * 128, :], o1[b][:], [("o1", b)], [("out", t)])
            S.wait_all("sp", [("out", t) for t in range(16)])
            for e in ("pe", "act", "dve", "pool"):
                S.wait_all(e, [("out", 14), ("out", 15)])
    return nc


_PROGRAM = None


def _own_rows(j):
    t = np.arange(16)
    return ((4 * t[:, None] + j) * 128 + np.arange(128)[None, :]).reshape(-1)


def kernel(x, norm1_g, w_in, a_qnorm_g, a_knorm_g, a_lambda, a_subln_g, t5_table,
           b_qnorm_g, b_knorm_g, b_rel_table, w_branch_a, w_branch_b, w_out, norm2_g,
           w_router_group, b_router_group, w_router_expert, b_router_expert,
           w_gate, w_up, w_down):
    global _PROGRAM
    f = lambda a: np.ascontiguousarray(np.asarray(a, dtype=np.float32))
    x = f(x)
    ident, J, bd, U = _consts()
    shared = {
        "w_in": f(w_in)[0], "norm1_g": f(norm1_g), "a_qnorm_g": f(a_qnorm_g), "a_knorm_g": f(a_knorm_g),
        "a_lambda": f(a_lambda).reshape(1, 256), "a_subln_g": f(a_subln_g), "t5_table": f(t5_table),
        "b_qnorm_g": f(b_qnorm_g), "b_knorm_g": f(b_knorm_g), "b_rel_table": f(b_rel_table)[0],
        "w_branch_a": f(w_branch_a)[0], "w_branch_b": f(w_branch_b)[0], "w_out": f(w_out)[0],
        "norm2_g": f(norm2_g), "w_router_group": f(w_router_group)[0], "b_router_group": f(b_router_group),
        "w_router_expert": f(w_router_expert)[0], "b_router_expert": f(b_router_expert),
        "w_gate": f(w_gate)[0], "w_up": f(w_up)[0], "w_down": f(w_down)[0],
        "c_ident": ident, "c_J": J, "c_bd": bd, "c_U": U,
    }
    xT = [np.ascontiguousarray(x[b].T) for b in range(2)]
    in_maps = []
    rows = []
    for c in range(NCORE):
        b, j = c // 4, c % 4
        r = _own_rows(j)
        rows.append(r)
        ohA, mA, ohB, mB = _structs(j)
        m = dict(shared)
        m.update({"xT_all": xT[b], "xT_own": np.ascontiguousarray(xT[b][:, r]), "x_own": np.ascontiguousarray(x[b][r]),
                  "c_ohA": ohA, "c_mA": mA, "c_ohB": ohB, "c_mB": mB})
        in_maps.append(m)
    if _PROGRAM is None:
        _PROGRAM = build_program()
    res = run_bass_kernel_spmd(_PROGRAM, in_maps, core_ids=list(range(NCORE)))
    if DEBUG is not None:
        return [r_["dbg"] for r_ in res.results]
    out = np.empty_like(x)
    for c in range(NCORE):
        out[c // 4, rows[c]] = res.results[c]["out_own"]
    return out
```
